# Optimizing a Trainium2 kernel written in Bass

```python
import math
import jax, jax.numpy as jnp
from jax import lax
import numpy as np


D_MODEL = 1024
BATCH = 4
SEQ = 8192
DEPTH = 2

ATT_HEADS = 4
ATT_DIM = 64
ATT_QK = ATT_HEADS * 2 * ATT_DIM
ATT_WIDTH = ATT_HEADS * 2 * ATT_DIM
ROT_DIM = ATT_DIM // 4
ROPE_THETA = 500000.0
Q_BLOCK = 128
GDN_HEADS = 4
GDN_DK = 128
GDN_DV = 128
GDN_WIDTH = GDN_HEADS * GDN_DV
GDN_CONV_CH = 2 * GDN_HEADS * GDN_DK + GDN_WIDTH
CONV_K = 4
GDN_CHUNK = 64
IN_COLS = 2 * ATT_QK + ATT_WIDTH + GDN_CONV_CH + GDN_WIDTH + 2 * GDN_HEADS + 2 * D_MODEL
D_FF = 2816
N_EXPERTS = 8
TOP_K = 2
D_EXPERT = 3584
MOE_BLOCK = 256
NORM_EPS = 1e-6
MAX_POS_OFFSET = 4096

kernel_name = 'hybrid_diffattn_gdn_moe_block'


def rms_norm(x, w):
    xf = x.astype(jnp.float32)
    y = xf * lax.rsqrt(jnp.mean(xf * xf, axis=-1, keepdims=True) + NORM_EPS)
    return (y * w.astype(jnp.float32)).astype(x.dtype)


def l2_normalize(x):
    return x * lax.rsqrt(jnp.sum(x * x, axis=-1, keepdims=True) + NORM_EPS)


def rope_tables(positions):
    inv_freq = ROPE_THETA ** (-jnp.arange(0, ROT_DIM, 2, dtype=jnp.float32) / ROT_DIM)
    ang = positions.astype(jnp.float32)[..., None] * inv_freq
    return jnp.cos(ang)[:, :, None, None, :], jnp.sin(ang)[:, :, None, None, :]


def partial_rope(x, cos, sin):
    half = ROT_DIM // 2
    x1, x2, rest = x[..., :half], x[..., half:ROT_DIM], x[..., ROT_DIM:]
    c, s = cos.astype(x.dtype), sin.astype(x.dtype)
    return jnp.concatenate([x1 * c - x2 * s, x2 * c + x1 * s, rest], axis=-1)


def diff_attention(q, k, v, lam):
    b_, s_, h_ = q.shape[:3]
    nqb = s_ // Q_BLOCK
    qb = jnp.moveaxis(q.reshape(b_, nqb, Q_BLOCK, h_, 2, ATT_DIM), 1, 0)
    key_pos = jnp.arange(s_)
    scale = ATT_DIM ** -0.5

    def block(args):
        q_blk, i = args
        s = jnp.einsum('bqhmd,bkhmd->bhmqk', q_blk, k).astype(jnp.float32) * scale
        q_pos = i * Q_BLOCK + jnp.arange(Q_BLOCK)
        causal = key_pos[None, :] <= q_pos[:, None]
        p = jax.nn.softmax(jnp.where(causal, s, -jnp.inf), axis=-1)
        a = p[:, :, 0] - lam * p[:, :, 1]
        return jnp.einsum('bhqk,bkhe->bqhe', a.astype(v.dtype), v)

    o = lax.map(block, (qb, jnp.arange(nqb)))
    return jnp.moveaxis(o, 0, 1).reshape(b_, s_, h_, 2 * ATT_DIM)


def causal_depthwise_conv(x, w):
    kw, ch = w.shape
    return lax.conv_general_dilated(x, w[:, None, :].astype(x.dtype), window_strides=(1,),
                                    padding=[(kw - 1, 0)], dimension_numbers=('NWC', 'WIO', 'NWC'),
                                    feature_group_count=ch)


def gated_delta_rule_chunked(q, k, v, g, beta):
    b_, s_, h_, dk = q.shape
    dv = v.shape[-1]
    c = GDN_CHUNK
    nc = s_ // c

    def to_chunks(t):
        return jnp.moveaxis(t.reshape((b_, nc, c) + t.shape[2:]), 3, 1)

    q, k, v, g, beta = (to_chunks(t) for t in (q, k, v, g, beta))
    gc = jnp.cumsum(g, axis=-1)
    idx = jnp.arange(c)
    incl = idx[:, None] >= idx[None, :]
    strict = idx[:, None] > idx[None, :]
    decay = jnp.exp(jnp.where(incl, gc[..., :, None] - gc[..., None, :], -jnp.inf))
    kb = k * beta[..., None]
    lmat = jnp.where(strict, jnp.einsum('bhncd,bhnjd->bhncj', kb, k) * decay, 0.0)
    rhs = jnp.concatenate([v * beta[..., None], kb * jnp.exp(gc)[..., None]], axis=-1)
    sol = lax.linalg.triangular_solve(lmat + jnp.eye(c, dtype=lmat.dtype), rhs, left_side=True,
                                      lower=True, unit_diagonal=True)
    u, w = sol[..., :dv], sol[..., dv:]
    a_qk = jnp.where(incl, jnp.einsum('bhncd,bhnjd->bhncj', q, k) * decay, 0.0)
    q_dec = q * jnp.exp(gc)[..., None]
    k_dec = k * jnp.exp(gc[..., -1:] - gc)[..., None]
    g_last = jnp.exp(gc[..., -1])
    xs = tuple(jnp.moveaxis(t, 2, 0) for t in (u, w, q_dec, k_dec, a_qk, g_last))

    def step(state, inp):
        u_c, w_c, qd, kd, aqk, gl = inp
        v_new = u_c - jnp.einsum('bhcd,bhde->bhce', w_c, state)
        o = jnp.einsum('bhcd,bhde->bhce', qd, state) + jnp.einsum('bhcj,bhje->bhce', aqk, v_new)
        state = state * gl[..., None, None] + jnp.einsum('bhcd,bhce->bhde', kd, v_new)
        return state, o

    state0 = jnp.zeros((b_, h_, dk, dv), jnp.float32)
    _, o = lax.scan(step, state0, xs)
    return jnp.transpose(o, (1, 0, 3, 2, 4)).reshape(b_, s_, h_, dv)


def hybrid_mixer(u, cos, sin, w_in, q_norm_w, k_norm_w, lam_vec, subln_w, conv_w, a_log, dt_bias,
                 gdn_norm_w, w_branch_a, w_branch_b, w_out, lam_init):
    b_, s_, _ = u.shape
    proj = u @ w_in
    widths = (ATT_QK, ATT_QK, ATT_WIDTH, GDN_CONV_CH, GDN_WIDTH, GDN_HEADS, GDN_HEADS, D_MODEL, D_MODEL)
    points, acc = [], 0
    for wd in widths[:-1]:
        acc += wd
        points.append(acc)
    aq, ak, av, bqkv, bz, bbeta, ba, gate_a, gate_b = jnp.split(proj, points, axis=-1)

    aq = partial_rope(rms_norm(aq.reshape(b_, s_, ATT_HEADS, 2, ATT_DIM), q_norm_w), cos, sin)
    ak = partial_rope(rms_norm(ak.reshape(b_, s_, ATT_HEADS, 2, ATT_DIM), k_norm_w), cos, sin)
    av = av.reshape(b_, s_, ATT_HEADS, 2 * ATT_DIM)
    lv = lam_vec.astype(jnp.float32)
    lam = jnp.exp(jnp.sum(lv[0] * lv[1])) - jnp.exp(jnp.sum(lv[2] * lv[3])) + lam_init
    oa = rms_norm(diff_attention(aq, ak, av, lam), subln_w) * (1.0 - lam_init)
    ya = oa.reshape(b_, s_, ATT_WIDTH) @ w_branch_a

    bqkv = jax.nn.silu(causal_depthwise_conv(bqkv, conv_w)).astype(jnp.float32)
    bq, bk, bv = jnp.split(bqkv, [GDN_HEADS * GDN_DK, 2 * GDN_HEADS * GDN_DK], axis=-1)
    bq = l2_normalize(bq.reshape(b_, s_, GDN_HEADS, GDN_DK)) * (GDN_DK ** -0.5)
    bk = l2_normalize(bk.reshape(b_, s_, GDN_HEADS, GDN_DK))
    bv = bv.reshape(b_, s_, GDN_HEADS, GDN_DV)
    beta = jax.nn.sigmoid(bbeta.astype(jnp.float32))
    g = -jnp.exp(a_log.astype(jnp.float32)) * jax.nn.softplus(ba.astype(jnp.float32) + dt_bias.astype(jnp.float32))
    ob = gated_delta_rule_chunked(bq, bk, bv, g, beta).astype(u.dtype)
    ob = rms_norm(ob, gdn_norm_w) * jax.nn.silu(bz.reshape(b_, s_, GDN_HEADS, GDN_DV))
    yb = ob.reshape(b_, s_, GDN_WIDTH) @ w_branch_b

    merged = jax.nn.sigmoid(gate_a) * ya + jax.nn.sigmoid(gate_b) * yb
    return merged @ w_out


def swiglu(h, w_gate_up, w_down):
    gt, up = jnp.split(h @ w_gate_up, 2, axis=-1)
    return (jax.nn.silu(gt) * up) @ w_down


def moe_swiglu(x, router_w, router_b, w_gate_up, w_down):
    b_, s_, d = x.shape
    t = b_ * s_
    xt = x.reshape(t, d)
    logits = xt.astype(jnp.float32) @ router_w.astype(jnp.float32) + router_b.astype(jnp.float32)
    top_val, top_idx = lax.top_k(logits, TOP_K)
    gates = jax.nn.softmax(top_val, axis=-1)
    n_assign = t * TOP_K
    e_flat = top_idx.reshape(n_assign)
    tok_flat = jnp.repeat(jnp.arange(t, dtype=jnp.int32), TOP_K)
    g_flat = gates.reshape(n_assign)
    order = jnp.argsort(e_flat)
    e_sorted, tok_sorted, g_sorted = e_flat[order], tok_flat[order], g_flat[order]
    counts = jnp.zeros((N_EXPERTS,), jnp.int32).at[e_flat].add(1)
    starts = jnp.cumsum(counts) - counts
    padded = (counts + MOE_BLOCK - 1) // MOE_BLOCK * MOE_BLOCK
    pad_end = jnp.cumsum(padded)
    pad_start = pad_end - padded
    dest = pad_start[e_sorted] + (jnp.arange(n_assign, dtype=jnp.int32) - starts[e_sorted])
    n_blocks = -(-n_assign // MOE_BLOCK) + N_EXPERTS
    p_len = n_blocks * MOE_BLOCK
    buf_tok = jnp.full((p_len,), t, jnp.int32).at[dest].set(tok_sorted)
    buf_g = jnp.zeros((p_len,), jnp.float32).at[dest].set(g_sorted)
    block_e = jnp.minimum(jnp.searchsorted(pad_end, jnp.arange(n_blocks) * MOE_BLOCK, side='right'),
                          N_EXPERTS - 1)
    x_pad = jnp.concatenate([xt, jnp.zeros((1, d), xt.dtype)], axis=0)
    xb = x_pad[buf_tok].reshape(n_blocks, MOE_BLOCK, d)

    def expert_block(args):
        xblk, e = args
        return swiglu(xblk, w_gate_up[e], w_down[e])

    yb = lax.map(expert_block, (xb, block_e)).reshape(p_len, d)
    y = jnp.zeros((t + 1, d), yb.dtype).at[buf_tok].add(yb * buf_g[:, None].astype(yb.dtype))
    return y[:t].reshape(b_, s_, d)


def setup_inputs(seed: int = 0) -> dict:
    key = jax.random.key(seed)
    ks = jax.random.split(key, 24)
    f32 = jnp.float32

    def nrm(k, shape, scale):
        return jax.random.normal(k, shape, f32) * scale

    n_dense = (DEPTH + 1) // 2
    n_moe = DEPTH // 2
    x = nrm(ks[0], (BATCH, SEQ, D_MODEL), 1.0)
    offsets = jax.random.randint(ks[1], (BATCH,), 0, MAX_POS_OFFSET, dtype=jnp.int32)
    positions = offsets[:, None] + jnp.arange(SEQ, dtype=jnp.int32)[None, :]
    norm_mix_w = 1.0 + nrm(ks[2], (DEPTH, D_MODEL), 0.02)
    w_in = nrm(ks[3], (DEPTH, D_MODEL, IN_COLS), D_MODEL ** -0.5)
    q_norm_w = 1.0 + nrm(ks[4], (DEPTH, ATT_DIM), 0.02)
    k_norm_w = 1.0 + nrm(ks[5], (DEPTH, ATT_DIM), 0.02)
    lam_vec = nrm(ks[6], (DEPTH, 4, ATT_DIM), 0.1)
    subln_w = 1.0 + nrm(ks[7], (DEPTH, 2 * ATT_DIM), 0.02)
    conv_w = nrm(ks[8], (DEPTH, CONV_K, GDN_CONV_CH), CONV_K ** -0.5)
    a_log = jnp.log(jax.random.uniform(ks[9], (DEPTH, GDN_HEADS), f32, 1.0, 16.0))
    dt = jnp.exp(jax.random.uniform(ks[10], (DEPTH, GDN_HEADS), f32, math.log(1e-3), math.log(1e-1)))
    dt_bias = dt + jnp.log(-jnp.expm1(-dt))
    gdn_norm_w = 1.0 + nrm(ks[11], (DEPTH, GDN_DV), 0.02)
    w_branch_a = nrm(ks[12], (DEPTH, ATT_WIDTH, D_MODEL), ATT_WIDTH ** -0.5)
    w_branch_b = nrm(ks[13], (DEPTH, GDN_WIDTH, D_MODEL), GDN_WIDTH ** -0.5)
    w_out = nrm(ks[14], (DEPTH, D_MODEL, D_MODEL), D_MODEL ** -0.5)
    norm_ffn_w = 1.0 + nrm(ks[15], (DEPTH, D_MODEL), 0.02)
    ffn_w_gate_up = nrm(ks[16], (n_dense, D_MODEL, 2 * D_FF), D_MODEL ** -0.5)
    ffn_w_down = nrm(ks[17], (n_dense, D_FF, D_MODEL), D_FF ** -0.5)
    router_w = nrm(ks[18], (n_moe, D_MODEL, N_EXPERTS), D_MODEL ** -0.5)
    router_b = nrm(ks[19], (n_moe, N_EXPERTS), 0.01)
    moe_w_gate_up = nrm(ks[20], (n_moe, N_EXPERTS, D_MODEL, 2 * D_EXPERT), D_MODEL ** -0.5)
    moe_w_down = nrm(ks[21], (n_moe, N_EXPERTS, D_EXPERT, D_MODEL), D_EXPERT ** -0.5)
    return {'x': x, 'positions': positions, 'norm_mix_w': norm_mix_w, 'w_in': w_in,
            'q_norm_w': q_norm_w, 'k_norm_w': k_norm_w, 'lam_vec': lam_vec, 'subln_w': subln_w,
            'conv_w': conv_w, 'a_log': a_log, 'dt_bias': dt_bias, 'gdn_norm_w': gdn_norm_w,
            'w_branch_a': w_branch_a, 'w_branch_b': w_branch_b, 'w_out': w_out,
            'norm_ffn_w': norm_ffn_w, 'ffn_w_gate_up': ffn_w_gate_up, 'ffn_w_down': ffn_w_down,
            'router_w': router_w, 'router_b': router_b, 'moe_w_gate_up': moe_w_gate_up,
            'moe_w_down': moe_w_down}


def reference(x, positions, norm_mix_w, w_in, q_norm_w, k_norm_w, lam_vec, subln_w, conv_w, a_log,
              dt_bias, gdn_norm_w, w_branch_a, w_branch_b, w_out, norm_ffn_w, ffn_w_gate_up,
              ffn_w_down, router_w, router_b, moe_w_gate_up, moe_w_down):
    cos, sin = rope_tables(positions)
    for layer in range(DEPTH):
        lam_init = 0.8 - 0.6 * math.exp(-0.3 * layer)
        h = rms_norm(x, norm_mix_w[layer])
        x = x + hybrid_mixer(h, cos, sin, w_in[layer], q_norm_w[layer], k_norm_w[layer], lam_vec[layer],
                             subln_w[layer], conv_w[layer], a_log[layer], dt_bias[layer],
                             gdn_norm_w[layer], w_branch_a[layer], w_branch_b[layer], w_out[layer],
                             lam_init)
        h = rms_norm(x, norm_ffn_w[layer])
        j = layer // 2
        if layer % 2 == 0:
            x = x + swiglu(h, ffn_w_gate_up[j], ffn_w_down[j])
        else:
            x = x + moe_swiglu(h, router_w[j], router_b[j], moe_w_gate_up[j], moe_w_down[j])
    return x
```

```python
import numpy as np
import concourse.bass as bass
import concourse.mybir as mybir

F32 = mybir.dt.float32
BF16 = mybir.dt.bfloat16
I32 = mybir.dt.int32
AF = mybir.ActivationFunctionType
ALU = mybir.AluOpType
AX = mybir.AxisListType


class Buf:
    __slots__ = ("name", "last_w", "readers", "dsem", "dcount")

    def __init__(self, name):
        self.name = name
        self.last_w = None
        self.readers = []
        self.dsem = None
        self.dcount = 0


class Op:
    __slots__ = ("eng", "fn", "idx", "signal", "dma", "waits_op", "waits_dma", "sigval")

    def __init__(self, eng, fn, idx, dma):
        self.eng = eng
        self.fn = fn
        self.idx = idx
        self.signal = False
        self.dma = dma
        self.waits_op = []
        self.waits_dma = []
        self.sigval = None


class _Rec:
    def __init__(self):
        self.call = None

    def __getattr__(self, name):
        def f(*a, **k):
            assert self.call is None
            self.call = (name, a, k)
            return self
        return f


class Prog:
    ENGS = ("pe", "act", "dve", "pool", "sp")

    def __init__(self, nc, same_engine_sync=True):
        self.nc = nc
        self.ops = {e: [] for e in self.ENGS}
        self.seen_op = {e: {p: -1 for p in self.ENGS} for e in self.ENGS}
        self.seen_dma = {e: {} for e in self.ENGS}
        self.same_engine_sync = same_engine_sync
        self.bufs = []
        self.nops = 0

    def buf(self, name):
        b = Buf(name)
        self.bufs.append(b)
        return b

    def add(self, eng, fn, reads=(), writes=(), dma=None):
        rec = _Rec()
        fn(rec)
        assert rec.call is not None
        call = rec.call
        fn = lambda e, call=call: getattr(e, call[0])(*call[1], **call[2])
        op = Op(eng, fn, len(self.ops[eng]), dma)
        deps = []
        for b in reads:
            if b.last_w is not None:
                deps.append(b.last_w)
        for b in writes:
            if b.last_w is not None:
                deps.append(b.last_w)
            deps.extend(b.readers)
        for d in deps:
            if d is op:
                continue
            if d.dma is not None:
                sb = d.dma
                val = 16 * sb.dcount
                if self.seen_dma[eng].get(id(sb), 0) >= val:
                    continue
                self.seen_dma[eng][id(sb)] = val
                op.waits_dma.append((sb, val))
            else:
                if d.eng == eng:
                    if eng == "pe" and dma is None:
                        continue
                    if not self.same_engine_sync and dma is None:
                        continue
                if self.seen_op[eng][d.eng] >= d.idx:
                    continue
                self.seen_op[eng][d.eng] = d.idx
                d.signal = True
                op.waits_op.append(d)
        for b in reads:
            b.readers.append(op)
        for b in writes:
            b.last_w = op
            b.readers = []
        if dma is not None:
            dma.dcount += 1
        self.ops[eng].append(op)
        self.nops += 1
        return op

    def emit(self, final_bufs=()):
        nc = self.nc
        for b in self.bufs:
            if b.dcount > 0:
                b.dsem = nc.alloc_semaphore("ds_" + b.name)
        EPOCH = 8000
        esem = {}
        for e in self.ENGS:
            c = 0
            ep = 0
            for op in self.ops[e]:
                if op.signal:
                    c += 1
                    if c > EPOCH:
                        ep += 1
                        c = 1
                    op.sigval = (ep, c)
            esem[e] = [nc.alloc_semaphore("es_%s_%d" % (e, i)) for i in range(ep + 1)]
        self.maxsig = {e: len(esem[e]) for e in self.ENGS}

        def run(e, eng):
            for op in self.ops[e]:
                best = {}
                for d in op.waits_op:
                    if d.eng not in best or best[d.eng] < d.sigval:
                        best[d.eng] = d.sigval
                for pe_, (ep, v) in best.items():
                    eng.wait_ge(esem[pe_][ep], v)
                for sb, val in op.waits_dma:
                    eng.wait_ge(sb.dsem, val)
                ins = op.fn(eng)
                if op.dma is not None:
                    ins.then_inc(op.dma.dsem, 16)
                if op.signal:
                    ins.then_inc(esem[e][op.sigval[0]], 1)
            if e == "sp":
                for b in final_bufs:
                    eng.wait_ge(b.dsem, 16 * b.dcount)

        with nc.Block() as block:
            @block.tensor
            def _(eng):
                run("pe", eng)

            @block.scalar
            def _(eng):
                run("act", eng)

            @block.vector
            def _(eng):
                run("dve", eng)

            @block.gpsimd
            def _(eng):
                run("pool", eng)

            @block.sync
            def _(eng):
                run("sp", eng)


import math
import numpy as np

EPS = 1e-6
TWO_PI = 2.0 * math.pi


class T:
    def __init__(self, P, name, shape, dtype, psum=False):
        nc = P.nc
        self.t = nc.alloc_psum_tensor("t_" + name, shape, dtype) if psum else nc.alloc_sbuf_tensor("t_" + name, shape, dtype)
        self.b = P.buf(name)

    def __getitem__(self, k):
        return self.t[k]


def _b(xs):
    return [x.b if hasattr(x, "b") else x for x in xs]


def bc(ap, shape):
    return ap.broadcast_to(list(shape))


NCONST = 128 * 4 + 8
NPRM = 8 + 24 + 128 + 256 + 1 + 1 + 2 + 2
WTM = 772
WCOLS = 772 + 1024


def host_consts():
    c = np.zeros((128, NCONST), np.float32)
    p = np.arange(128)[:, None]
    f = np.arange(128)[None, :]
    c[:, 0:128] = (p == f)
    c[:, 128:256] = (p <= f)
    c[:, 256:384] = (p < f)
    c[:, 384:512] = 1.0
    inv = (500000.0 ** (-np.arange(0, 16, 2, dtype=np.float32) / 16)).astype(np.float32)
    c[:, 512:520] = inv[None, :]
    return c


def build_A(nc, S, lam_init):
    NB = S // 512
    NT = S // 128
    P = Prog(nc)
    A = lambda eng, fn, reads=(), writes=(), dma=None: P.add(eng, fn, _b(reads), _b(writes), dma.b if hasattr(dma, "b") else dma)

    x_d = nc.dram_tensor("x", [S, 1024], F32, kind="ExternalInput").ap()
    pos_d = nc.dram_tensor("pos", [128, NT], I32, kind="ExternalInput").ap()
    w_d = nc.dram_tensor("w", [1024, WCOLS], F32, kind="ExternalInput").ap()
    prm_d = nc.dram_tensor("prm", [128, NPRM], F32, kind="ExternalInput").ap()
    cst_d = nc.dram_tensor("cst", [128, NCONST], F32, kind="ExternalInput").ap()
    oa_d = nc.dram_tensor("oaT", [2, 128, S], F32, kind="ExternalOutput").ap()
    ob_d = nc.dram_tensor("obT", [2, 128, S], F32, kind="ExternalOutput").ap()
    W = T(P, "W", [128, 8, WCOLS], BF16)
    cst = T(P, "cst", [128, NCONST], F32)
    prm = T(P, "prm", [128, NPRM], F32)
    posi = T(P, "posi", [128, NT], I32)
    ident_f = cst[:, 0:128]
    triU = cst[:, 128:256]
    strictU = cst[:, 256:384]
    ones_f = cst[:, 384:512]
    invf = cst[:, 512:520]
    cbf = T(P, "cbf", [128, 384], BF16)
    ident_b = cbf[:, 0:128]
    triU_b = cbf[:, 128:256]
    ones_b = cbf[:, 256:384]
    wn = prm[:, 0:8]
    cw = prm[:, 8:32]
    qkw = prm[:, 32:160]
    lamv = prm[:, 160:416]
    sublw = prm[:, 416:417]
    gdnw = prm[:, 417:418]
    alog = prm[:, 418:420]
    dtb = prm[:, 420:422]
    sm = T(P, "sm", [128, 32], F32)
    nlam = sm[:, 0:1]
    sw = sm[:, 1:2]
    gwn = sm[:, 2:3]
    nega = sm[:, 4:6]
    wqk2 = T(P, "wqk2", [128, 128], F32)
    epsT = T(P, "epsT", [128, 8], F32)
    cosT = T(P, "cosT", [128, NT, 8], F32)
    sinT = T(P, "sinT", [128, NT, 8], F32)
    kT = T(P, "kT", [128, 2, S], BF16)
    Vst = T(P, "Vst", [128, NT, 256], BF16)
    beta_all = T(P, "beta_all", [128, NT, 2], F32)
    nbeta_all = T(P, "nbeta_all", [128, NT, 2], F32)
    g_all = T(P, "g_all", [128, NT, 2], F32)
    Sst = [T(P, f"Sst{h}", [128, 128], F32) for h in range(2)]
    xc = [T(P, f"xc{j}", [128, 515], F32) for j in range(6)]

    xt = [T(P, f"xt{i}", [128, 1024], F32) for i in range(2)]
    xn = T(P, "xn", [128, 1024], BF16)
    st1 = T(P, "st1", [128, 8], F32)
    uT = T(P, "uT", [128, 8, 512], BF16)
    qT = T(P, "qT", [128, 2, 512], BF16)
    qb = T(P, "qb", [128, 512], BF16)
    rt = [T(P, f"rt{i}", [128, 8, 8], F32) for i in range(4)]
    st8 = T(P, "st8", [128, 16], F32)
    tb = T(P, "tb", [128, 8], F32)
    gq = [T(P, f"gq{j}", [128, 512], F32) for j in range(6)]
    szT = [T(P, f"szT{h}", [128, 512], F32) for h in range(2)]
    wk = [T(P, f"wk{i}", [128, 512], F32) for i in range(6)]
    tq = wk[3:6]
    pT = [T(P, f"pT{i}", [128, 512], BF16) for i in range(3)]
    outt = [T(P, f"outt{i}", [128, 512], F32) for i in range(2)]
    kgT = [T(P, f"kgT{h}", [128, 512], F32) for h in range(2)]
    qgT = [T(P, f"qgT{h}", [128, 512], F32) for h in range(2)]
    AqkT = [T(P, f"AqkT{h}", [128, 512], F32) for h in range(2)]
    Mm = [T(P, f"Mm{h}", [128, 512], F32) for h in range(2)]
    MmT = [T(P, f"MmT{h}", [128, 512], F32) for h in range(2)]
    Pm = [T(P, f"Pm{h}", [128, 512], F32) for h in range(2)]
    rr = [T(P, f"rr{h}", [128, 128], F32) for h in range(2)]
    vnew = [T(P, f"vnew{h}", [128, 128], F32) for h in range(2)]
    gcc = T(P, "gcc", [128, 8], F32)
    glast = T(P, "glast", [128, 8], F32)
    egl = T(P, "egl", [128, 8], F32)
    dkc = T(P, "dkc", [128, 8], F32)

    class V3:
        def __init__(self, t):
            self.t = t
            self.b = t.b
        def __getitem__(self, k):
            return self.t[:].rearrange("p (a b) -> p a b", a=4)[k]
    Gs, Dm, GTs = V3(wk[0]), V3(wk[1]), V3(wk[2])
    GT = [V3(wk[3]), V3(wk[3])]
    egc = [wk[4], wk[4]]
    vtm = Mm
    kd = MmT
    PB = [T(P, f"pb{i}", [128, 512], F32, psum=True) for i in range(8)]

    A("sp", lambda e: e.dma_start(out=cst[:], in_=cst_d), writes=[cst], dma=cst)
    A("sp", lambda e: e.dma_start(out=prm[:], in_=prm_d), writes=[prm], dma=prm)
    A("sp", lambda e: e.dma_start(out=posi[:], in_=pos_d), writes=[posi], dma=posi)
    wv = w_d.rearrange("(c p) n -> p c n", p=128)
    for c in range(8):
        A("pool", lambda e, c=c: e.dma_start(out=W[:, c, :], in_=wv[:, c, :]), writes=[W], dma=W)
    A("dve", lambda e: e.tensor_copy(out=cbf[:, 0:128], in_=ident_f), reads=[cst], writes=[cbf])
    A("dve", lambda e: e.tensor_copy(out=cbf[:, 128:256], in_=triU), reads=[cst], writes=[cbf])
    A("dve", lambda e: e.tensor_copy(out=cbf[:, 256:384], in_=ones_f), reads=[cst], writes=[cbf])
    for ci, cv in enumerate([EPS, 64 * EPS, 128 * EPS, 1.0, math.pi]):
        A("pool", lambda e, ci=ci, cv=cv: e.memset(epsT[:, ci:ci + 1], cv), writes=[epsT])
    A("dve", lambda e: e.tensor_tensor(out=tq[0][:, 0:64], in0=lamv[:, 0:64], in1=lamv[:, 64:128], op=ALU.mult), reads=[prm], writes=[tq[0]])
    A("dve", lambda e: e.tensor_tensor(out=tq[0][:, 64:128], in0=lamv[:, 128:192], in1=lamv[:, 192:256], op=ALU.mult), reads=[prm], writes=[tq[0]])
    A("dve", lambda e: e.tensor_reduce(out=sm[:, 8:10], in_=tq[0][:, 0:128].rearrange("p (a b) -> p a b", a=2), axis=AX.X, op=ALU.add), reads=[tq[0]], writes=[sm])
    A("act", lambda e: e.activation(out=sm[:, 10:12], in_=sm[:, 8:10], func=AF.Exp), reads=[sm], writes=[sm])
    A("act", lambda e: e.activation(out=sm[:, 6:8], in_=alog, func=AF.Exp), reads=[prm, sm], writes=[sm])
    A("dve", lambda e: e.tensor_tensor(out=sm[:, 12:13], in0=sm[:, 11:12], in1=sm[:, 10:11], op=ALU.subtract), reads=[sm], writes=[sm])
    A("dve", lambda e: e.tensor_scalar(out=nlam, in0=sm[:, 12:13], scalar1=-lam_init, scalar2=None, op0=ALU.add), reads=[sm], writes=[sm])
    A("dve", lambda e: e.tensor_scalar(out=sw, in0=sublw, scalar1=math.sqrt(128.0) * (1.0 - lam_init), scalar2=None, op0=ALU.mult), reads=[prm, sm], writes=[sm])
    A("dve", lambda e: e.tensor_scalar(out=gwn, in0=gdnw, scalar1=math.sqrt(128.0), scalar2=None, op0=ALU.mult), reads=[prm, sm], writes=[sm])
    A("dve", lambda e: e.tensor_scalar(out=nega, in0=sm[:, 6:8], scalar1=-1.0, scalar2=None, op0=ALU.mult), reads=[sm], writes=[sm])
    A("dve", lambda e: e.tensor_copy(out=wqk2[:, 0:64], in_=qkw[:, 0:64]), reads=[prm], writes=[wqk2])
    A("dve", lambda e: e.tensor_scalar(out=wqk2[:, 64:128], in0=qkw[:, 64:128], scalar1=8.0, scalar2=None, op0=ALU.mult), reads=[prm], writes=[wqk2])
    posf = tq[1]
    ang = tq[2]
    A("dve", lambda e: e.tensor_copy(out=posf[:, 0:NT], in_=posi[:]), reads=[posi], writes=[posf])
    NTC = min(NT, 64)
    assert NT <= 64
    A("dve", lambda e: e.tensor_tensor(out=ang[:, 0:NT * 8].rearrange("p (a b) -> p a b", b=8),
                                      in0=bc(posf[:, 0:NT].unsqueeze(2), [128, NT, 8]),
                                      in1=bc(invf.unsqueeze(1), [128, NT, 8]), op=ALU.mult), reads=[posf, cst], writes=[ang])
    C1 = 6.28125
    C2 = TWO_PI - C1
    NA = NT * 8

    def sin_of(src, dst, tA, tB):
        A("dve", lambda e: e.tensor_scalar(out=tB[:, 0:NA], in0=src[:, 0:NA], scalar1=1.0 / TWO_PI, scalar2=None, op0=ALU.mult), reads=[src], writes=[tB])
        A("dve", lambda e: e.tensor_copy(out=wk[0][:].bitcast(I32)[:, 0:NA], in_=tB[:, 0:NA]), reads=[tB], writes=[wk[0]])
        A("dve", lambda e: e.tensor_copy(out=tB[:, 0:NA], in_=wk[0][:].bitcast(I32)[:, 0:NA]), reads=[wk[0]], writes=[tB])
        A("dve", lambda e: e.scalar_tensor_tensor(out=tA[:, 0:NA], in0=tB[:, 0:NA], scalar=-C1, in1=src[:, 0:NA], op0=ALU.mult, op1=ALU.add), reads=[tB, src], writes=[tA])
        A("dve", lambda e: e.scalar_tensor_tensor(out=tA[:, 0:NA], in0=tB[:, 0:NA], scalar=-C2, in1=tA[:, 0:NA], op0=ALU.mult, op1=ALU.add), reads=[tB, tA], writes=[tA])
        A("dve", lambda e: e.tensor_scalar(out=tB[:, 0:NA], in0=tA[:, 0:NA], scalar1=math.pi, scalar2=-TWO_PI, op0=ALU.is_gt, op1=ALU.mult), reads=[tA], writes=[tB])
        A("dve", lambda e: e.tensor_tensor(out=tA[:, 0:NA], in0=tA[:, 0:NA], in1=tB[:, 0:NA], op=ALU.add), reads=[tA, tB], writes=[tA])
        A("dve", lambda e: e.tensor_scalar(out=tB[:, 0:NA], in0=tA[:, 0:NA], scalar1=-math.pi, scalar2=TWO_PI, op0=ALU.is_lt, op1=ALU.mult), reads=[tA], writes=[tB])
        A("dve", lambda e: e.tensor_tensor(out=tA[:, 0:NA], in0=tA[:, 0:NA], in1=tB[:, 0:NA], op=ALU.add), reads=[tA, tB], writes=[tA])
        A("act", lambda e: e.activation(out=dst[:].rearrange("p a b -> p (a b)"), in_=tA[:, 0:NA], func=AF.Sin), reads=[tA], writes=[dst])

    sin_of(ang, sinT, tq[0], tq[1])
    A("dve", lambda e: e.tensor_scalar(out=ang[:, 0:NA], in0=ang[:, 0:NA], scalar1=math.pi / 2, scalar2=None, op0=ALU.add), reads=[ang], writes=[ang])
    sin_of(ang, cosT, tq[0], tq[1])
    for h in range(2):
        A("pool", lambda e, h=h: e.memset(Sst[h][:], 0.0), writes=[Sst[h]])
    for j in range(6):
        A("pool", lambda e, j=j: e.memset(xc[j][:, 0:3], 0.0), writes=[xc[j]])

    def v4(ap, a):
        return ap.rearrange("p (a b) -> p a b", a=a)

    for t in range(NB):
        for s in range(4):
            tt = 4 * t + s
            X = xt[tt % 2]
            A("sp", lambda e, X=X, tt=tt: e.dma_start(out=X[:], in_=x_d[tt * 128:(tt + 1) * 128, :]), writes=[X], dma=X)
            A("dve", lambda e: e.memset(st1[:, 0:1], 0.0), writes=[st1])
            A("act", lambda e, X=X: e.activation(out=xn[:], in_=X[:], func=AF.Square, accum_out=st1[:, 0:1]), reads=[X, st1], writes=[xn, st1])
            A("act", lambda e: e.activation(out=st1[:, 1:2], in_=st1[:, 0:1], func=AF.Ln, scale=1.0 / 1024, bias=epsT[:, 0:1]), reads=[st1, epsT], writes=[st1])
            A("act", lambda e: e.activation(out=st1[:, 2:3], in_=st1[:, 1:2], func=AF.Exp, scale=-0.5), reads=[st1], writes=[st1])
            A("dve", lambda e, X=X: e.tensor_scalar(out=xn[:], in0=X[:], scalar1=st1[:, 2:3], scalar2=None, op0=ALU.mult), reads=[X, st1], writes=[xn])
            pbT = PB[s % 2]
            pbv = pbT[:].bitcast(BF16)
            for c in range(8):
                A("pe", lambda e, c=c, pbv=pbv: e.transpose(out=pbv[:, c * 128:(c + 1) * 128], in_=xn[:, c * 128:(c + 1) * 128], identity=ident_b), reads=[xn, cbf], writes=[pbT])
            A("dve", lambda e, pbv=pbv, s=s: e.tensor_tensor(out=uT[:, :, s * 128:(s + 1) * 128], in0=pbv.rearrange("p (a b) -> p a b", a=8),
                                                          in1=bc(wn.unsqueeze(2), [128, 8, 128]), op=ALU.mult), reads=[pbT, prm], writes=[uT])

        for s in range(4):
            tt = 4 * t + s
            bA = PB[2 + (s % 2) * 2]
            bB = PB[3 + (s % 2) * 2]
            for c in range(8):
                A("pe", lambda e, c=c, s=s, bA=bA: e.matmul(bA[:, 0:512], lhsT=uT[:, c, s * 128:(s + 1) * 128], rhs=W[:, c, 0:512], start=(c == 0), stop=(c == 7)), reads=[uT, W], writes=[bA])
            for c in range(8):
                A("pe", lambda e, c=c, s=s, bB=bB: e.matmul(bB[:, 0:260], lhsT=uT[:, c, s * 128:(s + 1) * 128], rhs=W[:, c, 512:772], start=(c == 0), stop=(c == 7)), reads=[uT, W], writes=[bB])
            A("act", lambda e, bB=bB, tt=tt: e.activation(out=Vst[:, tt, :], in_=bB[:, 0:256], func=AF.Copy), reads=[bB], writes=[Vst])
            A("act", lambda e, bB=bB: e.activation(out=tb[:, 0:2], in_=bB[:, 256:258], func=AF.Exp, scale=-1.0), reads=[bB], writes=[tb])
            A("dve", lambda e: e.tensor_scalar(out=tb[:, 2:4], in0=tb[:, 0:2], scalar1=1.0, scalar2=None, op0=ALU.add), reads=[tb], writes=[tb])
            A("dve", lambda e, tt=tt: e.reciprocal(out=beta_all[:, tt, :], in_=tb[:, 2:4]), reads=[tb], writes=[beta_all])
            A("dve", lambda e, tt=tt: e.tensor_scalar(out=nbeta_all[:, tt, :], in0=beta_all[:, tt, :], scalar1=-1.0, scalar2=None, op0=ALU.mult), reads=[beta_all], writes=[nbeta_all])
            A("dve", lambda e, bB=bB: e.tensor_tensor(out=tb[:, 4:6], in0=bB[:, 258:260], in1=dtb, op=ALU.add), reads=[bB, prm], writes=[tb])
            A("act", lambda e: e.activation(out=tb[:, 6:8], in_=tb[:, 4:6], func=AF.Exp), reads=[tb], writes=[tb])
            A("act", lambda e: e.activation(out=tb[:, 4:6], in_=tb[:, 6:8], func=AF.Ln, bias=epsT[:, 3:4]), reads=[tb, epsT], writes=[tb])
            A("dve", lambda e, tt=tt: e.tensor_tensor(out=g_all[:, tt, :], in0=tb[:, 4:6], in1=nega, op=ALU.mult), reads=[tb, sm], writes=[g_all])
            A("act", lambda e, bA=bA: e.activation(out=tq[0][:], in_=bA[:], func=AF.Square), reads=[bA], writes=[tq[0]])
            A("dve", lambda e: e.tensor_reduce(out=st8[:, 0:8], in_=v4(tq[0][:], 8), axis=AX.X, op=ALU.add), reads=[tq[0]], writes=[st8])
            A("act", lambda e: e.activation(out=st8[:, 0:8], in_=st8[:, 0:8], func=AF.Ln, bias=epsT[:, 1:2]), reads=[st8, epsT], writes=[st8])
            A("act", lambda e: e.activation(out=st8[:, 8:16], in_=st8[:, 0:8], func=AF.Exp, scale=-0.5), reads=[st8], writes=[st8])
            A("dve", lambda e, bA=bA: e.tensor_tensor(out=v4(tq[1][:], 8), in0=v4(bA[:], 8), in1=bc(st8[:, 8:16].unsqueeze(2), [128, 8, 64]), op=ALU.mult), reads=[bA, st8], writes=[tq[1]])
            A("dve", lambda e: e.tensor_tensor(out=tq[2][:].rearrange("p (a b c) -> p a b c", a=2, b=4),
                                              in0=tq[1][:].rearrange("p (a b c) -> p a b c", a=2, b=4),
                                              in1=bc(wqk2[:].rearrange("p (a c) -> p a c", a=2).unsqueeze(2), [128, 2, 4, 64]), op=ALU.mult), reads=[tq[1], wqk2], writes=[tq[2]])
            A("pool", lambda e: e.tensor_copy(out=qb[:], in_=tq[2][:]), reads=[tq[2]], writes=[qb])
            q3 = v4(tq[2][:], 8)
            x1 = q3[:, :, 0:8]
            x2 = q3[:, :, 8:16]
            cb = lambda tt=tt: bc(cosT[:, tt, :].unsqueeze(1), [128, 8, 8])
            sb_ = lambda tt=tt: bc(sinT[:, tt, :].unsqueeze(1), [128, 8, 8])
            A("dve", lambda e, cb=cb: e.tensor_tensor(out=rt[0][:], in0=x1, in1=cb(), op=ALU.mult), reads=[tq[2], cosT], writes=[rt[0]])
            A("dve", lambda e, sb_=sb_: e.tensor_tensor(out=rt[1][:], in0=x2, in1=sb_(), op=ALU.mult), reads=[tq[2], sinT], writes=[rt[1]])
            A("dve", lambda e, cb=cb: e.tensor_tensor(out=rt[2][:], in0=x2, in1=cb(), op=ALU.mult), reads=[tq[2], cosT], writes=[rt[2]])
            A("dve", lambda e, sb_=sb_: e.tensor_tensor(out=rt[3][:], in0=x1, in1=sb_(), op=ALU.mult), reads=[tq[2], sinT], writes=[rt[3]])
            qb3 = v4(qb[:], 8)
            A("dve", lambda e: e.tensor_tensor(out=qb3[:, :, 0:8], in0=rt[0][:], in1=rt[1][:], op=ALU.subtract), reads=[rt[0], rt[1], qb], writes=[qb])
            A("dve", lambda e: e.tensor_tensor(out=qb3[:, :, 8:16], in0=rt[2][:], in1=rt[3][:], op=ALU.add), reads=[rt[2], rt[3], qb], writes=[qb])
            pq = PB[s % 2]
            pqv = pq[:].bitcast(BF16)
            for i in range(4):
                A("pe", lambda e, i=i, pqv=pqv: e.transpose(out=pqv[:, i * 128:(i + 1) * 128], in_=qb[:, i * 128:(i + 1) * 128], identity=ident_b), reads=[qb, cbf], writes=[pq])
            A("act", lambda e, pqv=pqv, s=s: e.activation(out=qT[:, :, s * 128:(s + 1) * 128], in_=pqv[:, 0:256].rearrange("p (a b) -> p a b", a=2), func=AF.Copy), reads=[pq], writes=[qT])
            A("act", lambda e, pqv=pqv, tt=tt: e.activation(out=kT[:, :, tt * 128:(tt + 1) * 128], in_=pqv[:, 256:512].rearrange("p (a b) -> p a b", a=2), func=AF.Copy), reads=[pq], writes=[kT])

        for j in range(8):
            bk_ = PB[6 + (j % 2)]
            for c in range(8):
                A("pe", lambda e, c=c, j=j, bk_=bk_: e.matmul(bk_[:], lhsT=W[:, c, WTM + j * 128:WTM + (j + 1) * 128], rhs=uT[:, c, :], start=(c == 0), stop=(c == 7)), reads=[uT, W], writes=[bk_])
            if j < 6:
                X = xc[j]
                A("act", lambda e, X=X, bk_=bk_: e.activation(out=X[:, 3:515], in_=bk_[:], func=AF.Copy), reads=[bk_], writes=[X])
                y = wk[j % 2]
                A("pool", lambda e, X=X, y=y, j=j: e.tensor_scalar(out=y[:], in0=X[:, 0:512], scalar1=cw[:, j * 4:j * 4 + 1], scalar2=None, op0=ALU.mult), reads=[X, prm], writes=[y])
                for i in range(1, 4):
                    A("dve", lambda e, X=X, y=y, j=j, i=i: e.scalar_tensor_tensor(out=y[:], in0=X[:, i:i + 512], scalar=cw[:, j * 4 + i:j * 4 + i + 1], in1=y[:], op0=ALU.mult, op1=ALU.add), reads=[X, prm, y], writes=[y])
                A("pool", lambda e, X=X: e.tensor_copy(out=X[:, 0:3], in_=X[:, 512:515]), reads=[X], writes=[X])
                ee = wk[2 + (j % 2)]
                A("act", lambda e, y=y, ee=ee: e.activation(out=ee[:], in_=y[:], func=AF.Exp, scale=-1.0), reads=[y], writes=[ee])
                A("dve", lambda e, ee=ee: e.tensor_scalar(out=ee[:], in0=ee[:], scalar1=1.0, scalar2=None, op0=ALU.add), reads=[ee], writes=[ee])
                A("dve", lambda e, ee=ee: e.reciprocal(out=ee[:], in_=ee[:]), reads=[ee], writes=[ee])
                if j >= 4:
                    A("dve", lambda e, y=y, ee=ee, j=j: e.tensor_tensor(out=gq[j][:], in0=y[:], in1=ee[:], op=ALU.mult), reads=[y, ee], writes=[gq[j]])
                else:
                    sy = wk[4 + (j % 2)]
                    A("dve", lambda e, y=y, ee=ee, sy=sy: e.tensor_tensor(out=sy[:], in0=y[:], in1=ee[:], op=ALU.mult), reads=[y, ee], writes=[sy])
                    A("act", lambda e, sy=sy, ee=ee: e.activation(out=ee[:], in_=sy[:], func=AF.Square), reads=[sy], writes=[ee])
                    pn = PB[j % 2]
                    A("pe", lambda e, pn=pn, ee=ee: e.matmul(pn[:], lhsT=ones_f, rhs=ee[:], start=True, stop=True), reads=[ee, cst], writes=[pn])
                    A("act", lambda e, pn=pn, ee=ee: e.activation(out=ee[:], in_=pn[:], func=AF.Ln, bias=epsT[:, 0:1]), reads=[pn, epsT], writes=[ee])
                    A("act", lambda e, ee=ee: e.activation(out=ee[:], in_=ee[:], func=AF.Exp, scale=-0.5), reads=[ee], writes=[ee])
                    scl = (128.0 ** -0.5) if j < 2 else 1.0
                    A("dve", lambda e, sy=sy, ee=ee, j=j, scl=scl: e.scalar_tensor_tensor(out=gq[j][:], in0=sy[:], scalar=scl, in1=ee[:], op0=ALU.mult, op1=ALU.mult), reads=[sy, ee], writes=[gq[j]])
            else:
                h = j - 6
                ee = wk[2 + (j % 2)]
                A("act", lambda e, bk_=bk_, ee=ee: e.activation(out=ee[:], in_=bk_[:], func=AF.Exp, scale=-1.0), reads=[bk_], writes=[ee])
                A("dve", lambda e, ee=ee: e.tensor_scalar(out=ee[:], in0=ee[:], scalar1=1.0, scalar2=None, op0=ALU.add), reads=[ee], writes=[ee])
                A("dve", lambda e, ee=ee: e.reciprocal(out=ee[:], in_=ee[:]), reads=[ee], writes=[ee])
                A("dve", lambda e, bk_=bk_, ee=ee, h=h: e.tensor_tensor(out=szT[h][:], in0=bk_[:], in1=ee[:], op=ALU.mult), reads=[bk_, ee], writes=[szT[h]])

        nk = 4 * (t + 1)
        pti = 0
        for h in range(2):
            Ob = [PB[2], PB[4]]
            Db = [PB[3], PB[5]]
            for kt in range(nk):
                j = kt - 4 * t
                c0 = 128 * max(j, 0)
                for m in range(2):
                    sT = PB[(kt * 2 + m) % 2]
                    pt = pT[pti % 3]
                    pti += 1
                    A("pe", lambda e, sT=sT, m=m, h=h, kt=kt, c0=c0: e.matmul(sT[:, c0:512], lhsT=kT[m * 64:(m + 1) * 64, h, kt * 128:(kt + 1) * 128], rhs=qT[m * 64:(m + 1) * 64, h, c0:512], start=True, stop=True), reads=[kT, qT], writes=[sT])
                    A("act", lambda e, sT=sT, pt=pt, c0=c0: e.activation(out=pt[:, c0:512], in_=sT[:, c0:512], func=AF.Exp), reads=[sT], writes=[pt])
                    if j >= 0:
                        A("pool", lambda e, pt=pt, c0=c0: e.tensor_tensor(out=pt[:, c0:c0 + 128], in0=pt[:, c0:c0 + 128], in1=triU_b, op=ALU.mult), reads=[pt, cbf], writes=[pt])
                    A("pe", lambda e, pt=pt, m=m, h=h, kt=kt, c0=c0: e.matmul(Ob[m][:, c0:512], lhsT=Vst[:, kt, h * 128:(h + 1) * 128], rhs=pt[:, c0:512], start=(kt == 0), stop=(kt == nk - 1)), reads=[Vst, pt], writes=[Ob[m]])
                    A("pe", lambda e, pt=pt, m=m, kt=kt, c0=c0: e.matmul(Db[m][:, c0:512], lhsT=ones_b, rhs=pt[:, c0:512], start=(kt == 0), stop=(kt == nk - 1)), reads=[cbf, pt], writes=[Db[m]])
            A("dve", lambda e: e.reciprocal(out=wk[0][:], in_=Db[0][:]), reads=[Db[0]], writes=[wk[0]])
            A("dve", lambda e: e.tensor_tensor(out=wk[1][:], in0=Ob[0][:], in1=wk[0][:], op=ALU.mult), reads=[Ob[0], wk[0]], writes=[wk[1]])
            A("dve", lambda e: e.reciprocal(out=wk[2][:], in_=Db[1][:]), reads=[Db[1]], writes=[wk[2]])
            A("dve", lambda e: e.tensor_tensor(out=wk[3][:], in0=Ob[1][:], in1=wk[2][:], op=ALU.mult), reads=[Ob[1], wk[2]], writes=[wk[3]])
            A("dve", lambda e: e.scalar_tensor_tensor(out=wk[4][:], in0=wk[3][:], scalar=nlam, in1=wk[1][:], op0=ALU.mult, op1=ALU.add), reads=[wk[3], wk[1], sm], writes=[wk[4]])
            A("act", lambda e: e.activation(out=wk[5][:], in_=wk[4][:], func=AF.Square), reads=[wk[4]], writes=[wk[5]])
            pn = PB[6 + h]
            A("pe", lambda e, pn=pn: e.matmul(pn[:], lhsT=ones_f, rhs=wk[5][:], start=True, stop=True), reads=[wk[5], cst], writes=[pn])
            A("act", lambda e, pn=pn: e.activation(out=wk[0][:], in_=pn[:], func=AF.Ln, bias=epsT[:, 2:3]), reads=[pn, epsT], writes=[wk[0]])
            A("act", lambda e: e.activation(out=wk[0][:], in_=wk[0][:], func=AF.Exp, scale=-0.5), reads=[wk[0]], writes=[wk[0]])
            ot = outt[h]
            A("dve", lambda e, ot=ot: e.scalar_tensor_tensor(out=ot[:], in0=wk[4][:], scalar=sw, in1=wk[0][:], op0=ALU.mult, op1=ALU.mult), reads=[wk[4], wk[0], sm], writes=[ot])
            A("sp", lambda e, ot=ot, h=h, t=t: e.dma_start(out=oa_d[h, :, t * 512:(t + 1) * 512], in_=ot[:]), reads=[ot], dma=ot)

        gsl = lambda t=t: g_all[:, 4 * t:4 * t + 4, :]
        A("pe", lambda e: e.matmul(PB[0][:, 0:8], lhsT=triU, rhs=gsl().rearrange("p a b -> p (a b)"), start=True, stop=True), reads=[cst, g_all], writes=[PB[0]])
        A("dve", lambda e: e.tensor_copy(out=gcc[:], in_=PB[0][:, 0:8]), reads=[PB[0]], writes=[gcc])
        for h in range(2):
            gb = PB[2 + h]
            A("dve", lambda e, h=h, t=t: e.tensor_tensor(out=Gs[:], in0=bc(triU.unsqueeze(1), [128, 4, 128]), in1=bc(g_all[:, 4 * t:4 * t + 4, h:h + 1], [128, 4, 128]), op=ALU.mult), reads=[cst, g_all], writes=[Gs])
            A("pe", lambda e, gb=gb: e.matmul(gb[:], lhsT=ones_f, rhs=Gs[:].rearrange("p a b -> p (a b)"), start=True, stop=True), reads=[cst, Gs], writes=[gb])
            A("act", lambda e, gb=gb, h=h: e.activation(out=egc[h][:], in_=gb[:], func=AF.Exp), reads=[gb], writes=[egc[h]])
            A("dve", lambda e, h=h: e.tensor_tensor(out=kgT[h][:], in0=gq[2 + h][:], in1=egc[h][:], op=ALU.mult), reads=[gq[2 + h], egc[h]], writes=[kgT[h]])
            A("dve", lambda e, h=h: e.tensor_tensor(out=qgT[h][:], in0=gq[h][:], in1=egc[h][:], op=ALU.mult), reads=[gq[h], egc[h]], writes=[qgT[h]])
            for s in range(4):
                i = s * 2 + h
                A("dve", lambda e, gb=gb, s=s, i=i: e.tensor_scalar(out=Dm[:, s, :], in0=gb[:, s * 128:(s + 1) * 128], scalar1=gcc[:, i:i + 1], scalar2=0.0, op0=ALU.subtract, op1=ALU.min), reads=[gb, gcc], writes=[Dm])
            A("dve", lambda e, gb=gb, h=h: e.tensor_copy(out=glast[:, h * 4:h * 4 + 4], in_=v4(gb[:], 4)[:, :, 127]), reads=[gb], writes=[glast])
            A("act", lambda e: e.activation(out=Dm[:].rearrange("p a b -> p (a b)"), in_=Dm[:].rearrange("p a b -> p (a b)"), func=AF.Exp), reads=[Dm], writes=[Dm])
            A("dve", lambda e, h=h: e.tensor_tensor(out=GT[h][:], in0=Dm[:], in1=bc(triU.unsqueeze(1), [128, 4, 128]), op=ALU.mult), reads=[Dm, cst], writes=[GT[h]])
            A("dve", lambda e: e.tensor_tensor(out=GTs[:], in0=Dm[:], in1=bc(strictU.unsqueeze(1), [128, 4, 128]), op=ALU.mult), reads=[Dm, cst], writes=[GTs])
            A("act", lambda e, h=h: e.activation(out=egl[:, h * 4:h * 4 + 4], in_=glast[:, h * 4:h * 4 + 4], func=AF.Exp), reads=[glast], writes=[egl])
            pkk = PB[4 + h]
            pqk = PB[6 + h]
            for s in range(4):
                sl = slice(s * 128, (s + 1) * 128)
                A("pe", lambda e, pkk=pkk, sl=sl, h=h: e.matmul(pkk[:, sl], lhsT=gq[2 + h][:, sl], rhs=gq[2 + h][:, sl], start=True, stop=True), reads=[gq[2 + h]], writes=[pkk])
                A("pe", lambda e, pqk=pqk, sl=sl, h=h: e.matmul(pqk[:, sl], lhsT=gq[2 + h][:, sl], rhs=gq[h][:, sl], start=True, stop=True), reads=[gq[2 + h], gq[h]], writes=[pqk])
            for s in range(4):
                sl = slice(s * 128, (s + 1) * 128)
                A("dve", lambda e, pkk=pkk, sl=sl, s=s, h=h, t=t: e.scalar_tensor_tensor(out=Mm[h][:, sl], in0=pkk[:, sl], scalar=nbeta_all[:, 4 * t + s, h:h + 1], in1=GTs[:, s, :], op0=ALU.mult, op1=ALU.mult), reads=[pkk, nbeta_all, GTs], writes=[Mm[h]])
            A("dve", lambda e, pqk=pqk, h=h: e.tensor_tensor(out=AqkT[h][:], in0=pqk[:], in1=GT[h][:].rearrange("p a b -> p (a b)"), op=ALU.mult), reads=[pqk, GT[h]], writes=[AqkT[h]])
            for s in range(4):
                i = s * 2 + h
                A("act", lambda e, i=i, s=s, h=h: e.activation(out=dkc[:, i:i + 1], in_=gcc[:, i:i + 1], func=AF.Exp, scale=-1.0, bias=glast[:, h * 4 + s:h * 4 + s + 1]), reads=[gcc, glast], writes=[dkc])
            pa = PB[h]
            for s in range(4):
                sl = slice(s * 128, (s + 1) * 128)
                A("pe", lambda e, pa=pa, sl=sl, h=h: e.transpose(out=pa[:, sl], in_=Mm[h][:, sl], identity=ident_f), reads=[Mm[h], cst], writes=[pa])
            A("act", lambda e, pa=pa, h=h: e.activation(out=MmT[h][:], in_=pa[:], func=AF.Copy), reads=[pa], writes=[MmT[h]])
            A("dve", lambda e, h=h: e.tensor_tensor(out=v4(Pm[h][:], 4), in0=v4(Mm[h][:], 4), in1=bc(ident_f.unsqueeze(1), [128, 4, 128]), op=ALU.add), reads=[Mm[h], cst], writes=[Pm[h]])
        for l in range(1, 7):
            for h in range(2):
                last = (l == 6)
                p1, p2, p3 = PB[h * 3], PB[h * 3 + 1], PB[h * 3 + 2]
                for s in range(4):
                    sl = slice(s * 128, (s + 1) * 128)
                    if not last:
                        A("pe", lambda e, p1=p1, sl=sl, h=h: e.matmul(p1[:, sl], lhsT=MmT[h][:, sl], rhs=Mm[h][:, sl], start=True, stop=True), reads=[MmT[h], Mm[h]], writes=[p1])
                    A("pe", lambda e, p2=p2, sl=sl, h=h: e.matmul(p2[:, sl], lhsT=Mm[h][:, sl], rhs=MmT[h][:, sl], start=True, stop=True), reads=[MmT[h], Mm[h]], writes=[p2])
                if not last:
                    A("act", lambda e, p1=p1, h=h: e.activation(out=Mm[h][:], in_=p1[:], func=AF.Copy), reads=[p1], writes=[Mm[h]])
                A("act", lambda e, p2=p2, h=h: e.activation(out=MmT[h][:], in_=p2[:], func=AF.Copy), reads=[p2], writes=[MmT[h]])
                for s in range(4):
                    sl = slice(s * 128, (s + 1) * 128)
                    A("pe", lambda e, p3=p3, sl=sl, h=h: e.matmul(p3[:, sl], lhsT=MmT[h][:, sl], rhs=Pm[h][:, sl], start=True, stop=True), reads=[MmT[h], Pm[h]], writes=[p3])
                A("dve", lambda e, p3=p3, h=h: e.tensor_tensor(out=Pm[h][:], in0=Pm[h][:], in1=p3[:], op=ALU.add), reads=[p3, Pm[h]], writes=[Pm[h]])
        for h in range(2):
            pv = PB[2 + h]
            for s in range(4):
                sl = slice(s * 128, (s + 1) * 128)
                A("pe", lambda e, pv=pv, sl=sl, h=h: e.transpose(out=pv[:, sl], in_=gq[4 + h][:, sl], identity=ident_f), reads=[gq[4 + h], cst], writes=[pv])
            A("act", lambda e, pv=pv, h=h: e.activation(out=vtm[h][:], in_=pv[:], func=AF.Copy), reads=[pv], writes=[vtm[h]])
            pk2 = PB[4 + h]
            for s in range(4):
                sl = slice(s * 128, (s + 1) * 128)
                A("pe", lambda e, pk2=pk2, sl=sl, h=h: e.transpose(out=pk2[:, sl], in_=gq[2 + h][:, sl], identity=ident_f), reads=[gq[2 + h], cst], writes=[pk2])
            for s in range(4):
                sl = slice(s * 128, (s + 1) * 128)
                i = s * 2 + h
                A("dve", lambda e, pk2=pk2, sl=sl, i=i, h=h: e.tensor_scalar(out=kd[h][:, sl], in0=pk2[:, sl], scalar1=dkc[:, i:i + 1], scalar2=None, op0=ALU.mult), reads=[pk2, dkc], writes=[kd[h]])
        po = [PB[6], PB[7]]
        for s in range(4):
            sl = slice(s * 128, (s + 1) * 128)
            for h in range(2):
                i = s * 2 + h
                q1 = PB[h * 3]
                q2 = PB[h * 3 + 1]
                q3_ = PB[h * 3 + 2]
                A("pe", lambda e, q1=q1, sl=sl, h=h: e.matmul(q1[:, 0:128], lhsT=kgT[h][:, sl], rhs=Sst[h][:], start=True, stop=True), reads=[kgT[h], Sst[h]], writes=[q1])
                A("dve", lambda e, q1=q1, sl=sl, h=h: e.tensor_tensor(out=rr[h][:], in0=vtm[h][:, sl], in1=q1[:, 0:128], op=ALU.subtract), reads=[vtm[h], q1], writes=[rr[h]])
                A("pe", lambda e, q2=q2, sl=sl, h=h: e.matmul(q2[:, 0:128], lhsT=Pm[h][:, sl], rhs=rr[h][:], start=True, stop=True), reads=[Pm[h], rr[h]], writes=[q2])
                A("dve", lambda e, q2=q2, h=h, s=s, t=t: e.tensor_scalar(out=vnew[h][:], in0=q2[:, 0:128], scalar1=beta_all[:, 4 * t + s, h:h + 1], scalar2=None, op0=ALU.mult), reads=[q2, beta_all], writes=[vnew[h]])
                A("pe", lambda e, sl=sl, h=h: e.matmul(po[h][:, sl], lhsT=Sst[h][:], rhs=qgT[h][:, sl], start=True, stop=False), reads=[Sst[h], qgT[h]], writes=[po[h]])
                A("pe", lambda e, sl=sl, h=h: e.matmul(po[h][:, sl], lhsT=vnew[h][:], rhs=AqkT[h][:, sl], start=False, stop=True), reads=[vnew[h], AqkT[h]], writes=[po[h]])
                A("pe", lambda e, q3_=q3_, sl=sl, h=h: e.matmul(q3_[:, 0:128], lhsT=kd[h][:, sl], rhs=vnew[h][:], start=True, stop=True), reads=[kd[h], vnew[h]], writes=[q3_])
                A("dve", lambda e, q3_=q3_, h=h, s=s: e.scalar_tensor_tensor(out=Sst[h][:], in0=Sst[h][:], scalar=egl[:, h * 4 + s:h * 4 + s + 1], in1=q3_[:, 0:128], op0=ALU.mult, op1=ALU.add), reads=[Sst[h], egl, q3_], writes=[Sst[h]])
        for h in range(2):
            A("act", lambda e, h=h: e.activation(out=wk[h][:], in_=po[h][:], func=AF.Square), reads=[po[h]], writes=[wk[h]])
            pn = PB[h * 3]
            A("pe", lambda e, pn=pn, h=h: e.matmul(pn[:], lhsT=ones_f, rhs=wk[h][:], start=True, stop=True), reads=[wk[h], cst], writes=[pn])
            A("act", lambda e, pn=pn, h=h: e.activation(out=wk[2 + h][:], in_=pn[:], func=AF.Ln, bias=epsT[:, 2:3]), reads=[pn, epsT], writes=[wk[2 + h]])
            A("act", lambda e, h=h: e.activation(out=wk[2 + h][:], in_=wk[2 + h][:], func=AF.Exp, scale=-0.5), reads=[wk[2 + h]], writes=[wk[2 + h]])
            A("dve", lambda e, h=h: e.scalar_tensor_tensor(out=wk[4 + h][:], in0=po[h][:], scalar=gwn, in1=wk[2 + h][:], op0=ALU.mult, op1=ALU.mult), reads=[po[h], wk[2 + h], sm], writes=[wk[4 + h]])
            ot = outt[h]
            A("dve", lambda e, ot=ot, h=h: e.tensor_tensor(out=ot[:], in0=wk[4 + h][:], in1=szT[h][:], op=ALU.mult), reads=[wk[4 + h], szT[h]], writes=[ot])
            A("sp", lambda e, ot=ot, h=h, t=t: e.dma_start(out=ob_d[h, :, t * 512:(t + 1) * 512], in_=ot[:]), reads=[ot], dma=ot)

    P.emit(final_bufs=[outt[0].b, outt[1].b])
    return P


def host_inputs_A(inp, layer, b, hh, S):
    w_in = inp["w_in"][layer]
    cols = []
    o = 0
    aq, ak, av, bq, bk, bv, bz, bbeta, ba = 0, 512, 1024, 1536, 2048, 2560, 3072, 3584, 3588
    r = lambda base, n=256: np.arange(base + hh * n, base + (hh + 1) * n)
    idx = np.concatenate([r(aq), r(ak), r(av), r(bbeta, 2), r(ba, 2), r(bq), r(bk), r(bv), r(bz)])
    Wc = np.ascontiguousarray(w_in[:, idx])
    prm = np.zeros((128, NPRM), np.float32)
    prm[:, 0:8] = inp["norm_mix_w"][layer].reshape(8, 128).T
    cwf = inp["conv_w"][layer]
    for j in range(6):
        base = (j // 2) * 512 + hh * 256 + (j % 2) * 128
        prm[:, 8 + j * 4:8 + j * 4 + 4] = cwf[:, base:base + 128].T
    prm[:, 32:96] = inp["q_norm_w"][layer][None, :]
    prm[:, 96:160] = inp["k_norm_w"][layer][None, :]
    prm[:, 160:416] = inp["lam_vec"][layer].reshape(1, 256)
    prm[:, 416] = inp["subln_w"][layer]
    prm[:, 417] = inp["gdn_norm_w"][layer]
    prm[:, 418:420] = inp["a_log"][layer][None, 2 * hh:2 * hh + 2]
    prm[:, 420:422] = inp["dt_bias"][layer][None, 2 * hh:2 * hh + 2]
    pos = np.ascontiguousarray(inp["positions"][b, :S].reshape(S // 128, 128).T).astype(np.int32)
    return {"w": Wc, "prm": prm, "pos": pos, "cst": host_consts()}


import math
import numpy as np

NPRMB = 24


def build_B(nc, NTOK, moe, F, E):
    PASS = 1024
    NH = NTOK // PASS
    P = Prog(nc)
    A = lambda eng, fn, reads=(), writes=(), dma=None: P.add(eng, fn, _b(reads), _b(writes), dma.b if isinstance(dma, T) else dma)

    x_d = nc.dram_tensor("x", [NTOK, 1024], F32, kind="ExternalInput").ap()
    oa_d = nc.dram_tensor("oaT", [4, 128, NTOK], F32, kind="ExternalInput").ap()
    ob_d = nc.dram_tensor("obT", [4, 128, NTOK], F32, kind="ExternalInput").ap()
    wg_d = nc.dram_tensor("wg", [1024, 2048], F32, kind="ExternalInput").ap()
    wa_d = nc.dram_tensor("wa", [512, 1024], F32, kind="ExternalInput").ap()
    wb_d = nc.dram_tensor("wb", [512, 1024], F32, kind="ExternalInput").ap()
    wo_d = nc.dram_tensor("wo", [1024, 1024], F32, kind="ExternalInput").ap()
    prm_d = nc.dram_tensor("prm", [128, NPRMB], F32, kind="ExternalInput").ap()
    idn_d = nc.dram_tensor("idn", [128, 128], F32, kind="ExternalInput").ap()
    wgu_d = nc.dram_tensor("wgu", [E, 1024, 2 * F], F32, kind="ExternalInput").ap()
    wd_d = nc.dram_tensor("wd", [E, F, 1024], F32, kind="ExternalInput").ap()
    if moe:
        rw_d = nc.dram_tensor("rw", [1024, 8], F32, kind="ExternalInput").ap()
    out_d = nc.dram_tensor("xo", [NTOK, 1024], F32, kind="ExternalOutput").ap()

    U = nc.alloc_sbuf_tensor("t_U", [128, 32768], BF16)
    class V:
        def __init__(self, name, ap):
            self.ap = ap
            self.b = P.buf(name)
        def __getitem__(self, k):
            return self.ap[k]
    Wg = V("Wg", U[:, 0:16384].rearrange("p (c n) -> p c n", c=8))
    Wo = V("Wo", U[:, 16384:24576].rearrange("p (c n) -> p c n", c=8))
    Wa = V("Wa", U[:, 24576:28672].rearrange("p (c n) -> p c n", c=4))
    Wb = V("Wb", U[:, 28672:32768].rearrange("p (c n) -> p c n", c=4))
    Wgu = [V(f"Wgu{i}", U[:, i * 8192:(i + 1) * 8192].rearrange("p (c n) -> p c n", c=8)) for i in range(2)]
    Wd = [V(f"Wd{i}", U[:, 16384 + i * 4096:16384 + (i + 1) * 4096].rearrange("p (c n) -> p c n", c=4)) for i in range(2)]
    actT = [V(f"actT{i}", U[:, 24576 + i * 2048:24576 + (i + 1) * 2048].rearrange("p (c n) -> p c n", c=4)) for i in range(2)]
    b1v = [Wg, Wo, Wa, Wb]
    ffv = Wgu + Wd + actT

    def _b2(xs):
        return [x.b if isinstance(x, (T, V)) else x for x in xs]
    A = lambda eng, fn, reads=(), writes=(), dma=None: P.add(eng, fn, _b2(reads), _b2(writes), dma.b if isinstance(dma, (T, V)) else dma)

    def alias_barrier(new, old):
        pend = []
        for o in old:
            if o.b.last_w is not None:
                pend.append(o.b.last_w)
            pend.extend(o.b.readers)
        for n in new:
            n.b.readers = list(n.b.readers) + pend

    acc = [T(P, f"acc{i}", [128, 1024], F32) for i in range(8)]
    hT = T(P, "hT", [128, 8, PASS], BF16)
    X = [T(P, f"X{i}", [128, 1024], F32) for i in range(4)]
    xn = T(P, "xn", [128, 1024], BF16)
    junk = T(P, "junk", [128, 1024], BF16)
    hnf = T(P, "hnf", [128, 1024], F32)
    hTf = T(P, "hTf", [128, 8, 128], F32)
    uT = T(P, "uT", [128, 8, 512], BF16)
    mT = T(P, "mT", [128, 8, 512], BF16)
    oab = T(P, "oab", [128, 8, 512], BF16)
    tf = [T(P, f"tf{i}", [128, 512], F32) for i in range(6)]
    st1 = T(P, "st1", [128, 8], F32)
    prm = T(P, "prm", [128, NPRMB], F32)
    idf = T(P, "idf", [128, 128], F32)
    idb = T(P, "idb", [128, 128], BF16)
    epsT = T(P, "epsT", [128, 8], F32)
    gates = T(P, "gates", [128, 8, 8], F32)
    rt_ = T(P, "rt_", [128, 64], F32)
    if moe:
        Rw = T(P, "Rw", [128, 8, 8], F32)
    PB = [T(P, f"pb{i}", [128, 512], F32, psum=True) for i in range(8)]
    wn_mix = prm[:, 0:8]
    wn_ffn = prm[:, 8:16]
    rb = prm[:, 16:24]

    A("sp", lambda e: e.dma_start(out=prm[:], in_=prm_d), writes=[prm], dma=prm)
    A("sp", lambda e: e.dma_start(out=idf[:], in_=idn_d), writes=[idf], dma=idf)
    if moe:
        A("sp", lambda e: e.dma_start(out=Rw[:], in_=rw_d.rearrange("(c p) n -> p c n", p=128)), writes=[Rw], dma=Rw)
    A("dve", lambda e: e.tensor_copy(out=idb[:], in_=idf[:]), reads=[idf], writes=[idb])
    for ci, cv in enumerate([EPS, 1.0]):
        A("dve", lambda e, ci=ci, cv=cv: e.memset(epsT[:, ci:ci + 1], cv), writes=[epsT])

    def rmsnorm_stats(src_ap, srcT):
        A("act", lambda e: e.activation(out=junk[:], in_=src_ap, func=AF.Square, accum_out=st1[:, 0:1]), reads=[srcT], writes=[junk, st1])
        A("act", lambda e: e.activation(out=st1[:, 1:2], in_=st1[:, 0:1], func=AF.Ln, scale=1.0 / 1024, bias=epsT[:, 0:1]), reads=[st1, epsT], writes=[st1])
        A("act", lambda e: e.activation(out=st1[:, 2:3], in_=st1[:, 1:2], func=AF.Exp, scale=-0.5), reads=[st1], writes=[st1])

    def sigmoid_from_psum(ps, dst):
        A("act", lambda e: e.activation(out=dst[:], in_=ps[:], func=AF.Exp, scale=-1.0), reads=[ps], writes=[dst])
        A("act", lambda e: e.activation(out=dst[:], in_=dst[:], func=AF.Ln, bias=epsT[:, 1:2]), reads=[dst, epsT], writes=[dst])
        A("act", lambda e: e.activation(out=dst[:], in_=dst[:], func=AF.Exp, scale=-1.0), reads=[dst], writes=[dst])

    nchunk = (F + 511) // 512
    for hp in range(NH):
        alias_barrier(b1v, ffv)
        A("pool", lambda e: e.dma_start(out=Wg[:], in_=wg_d.rearrange("(c p) n -> p c n", p=128)), writes=[Wg], dma=Wg)
        A("pool", lambda e: e.dma_start(out=Wa[:], in_=wa_d.rearrange("(c p) n -> p c n", p=128)), writes=[Wa], dma=Wa)
        A("pool", lambda e: e.dma_start(out=Wb[:], in_=wb_d.rearrange("(c p) n -> p c n", p=128)), writes=[Wb], dma=Wb)
        A("pool", lambda e: e.dma_start(out=Wo[:], in_=wo_d.rearrange("(c p) n -> p c n", p=128)), writes=[Wo], dma=Wo)
        for blk in range(2):
            tb = hp * 2 + blk
            t0 = tb * 512
            for s in range(4):
                A("sp", lambda e: e.dma_start(out=X[s][:], in_=x_d[t0 + s * 128:t0 + (s + 1) * 128, :]), writes=[X[s]], dma=X[s])
            A("pool", lambda e: e.dma_start(out=oab[:, 0:4, :], in_=oa_d[:, :, t0:t0 + 512].rearrange("h p n -> p h n")), writes=[oab], dma=oab)
            A("pool", lambda e: e.dma_start(out=oab[:, 4:8, :], in_=ob_d[:, :, t0:t0 + 512].rearrange("h p n -> p h n")), writes=[oab], dma=oab)
            for s in range(4):
                rmsnorm_stats(X[s][:], X[s])
                A("dve", lambda e: e.tensor_scalar(out=xn[:], in0=X[s][:], scalar1=st1[:, 2:3], scalar2=None, op0=ALU.mult), reads=[X[s], st1], writes=[xn])
                pbT = PB[s % 2]
                pbv = pbT[:].bitcast(BF16)
                for c in range(8):
                    A("pe", lambda e: e.transpose(out=pbv[:, c * 128:(c + 1) * 128], in_=xn[:, c * 128:(c + 1) * 128], identity=idb[:]), reads=[xn, idb], writes=[pbT])
                A("dve", lambda e: e.tensor_tensor(out=uT[:, :, s * 128:(s + 1) * 128], in0=pbv.rearrange("p (a b) -> p a b", a=8),
                                                  in1=bc(wn_mix.unsqueeze(2), [128, 8, 128]), op=ALU.mult), reads=[pbT, prm], writes=[uT])
            for cc in range(8):
                pga, pgb, pya, pyb = PB[2 + (cc % 2) * 3], PB[3 + (cc % 2) * 3], PB[4 + (cc % 2) * 3], PB[(cc % 2)]
                csl = slice(cc * 128, (cc + 1) * 128)
                for c in range(8):
                    A("pe", lambda e: e.matmul(pga[:], lhsT=Wg[:, c, cc * 128:(cc + 1) * 128], rhs=uT[:, c, :], start=(c == 0), stop=(c == 7)), reads=[Wg, uT], writes=[pga])
                for c in range(8):
                    A("pe", lambda e: e.matmul(pgb[:], lhsT=Wg[:, c, 1024 + cc * 128:1024 + (cc + 1) * 128], rhs=uT[:, c, :], start=(c == 0), stop=(c == 7)), reads=[Wg, uT], writes=[pgb])
                for h in range(4):
                    A("pe", lambda e: e.matmul(pya[:], lhsT=Wa[:, h, csl], rhs=oab[:, h, :], start=(h == 0), stop=(h == 3)), reads=[Wa, oab], writes=[pya])
                for h in range(4):
                    A("pe", lambda e: e.matmul(pyb[:], lhsT=Wb[:, h, csl], rhs=oab[:, 4 + h, :], start=(h == 0), stop=(h == 3)), reads=[Wb, oab], writes=[pyb])
                sga, sgb = tf[(cc % 2) * 2], tf[(cc % 2) * 2 + 1]
                sigmoid_from_psum(pga, sga)
                sigmoid_from_psum(pgb, sgb)
                A("dve", lambda e: e.tensor_tensor(out=sga[:], in0=sga[:], in1=pya[:], op=ALU.mult), reads=[sga, pya], writes=[sga])
                A("dve", lambda e: e.tensor_tensor(out=sgb[:], in0=sgb[:], in1=pyb[:], op=ALU.mult), reads=[sgb, pyb], writes=[sgb])
                A("pool", lambda e: e.tensor_tensor(out=mT[:, cc, :], in0=sga[:], in1=sgb[:], op=ALU.add), reads=[sga, sgb], writes=[mT])
            for s in range(4):
                lt = blk * 4 + s
                for half in range(2):
                    px = PB[2 + half]
                    for cc in range(8):
                        A("pe", lambda e: e.matmul(px[:], lhsT=mT[:, cc, s * 128:(s + 1) * 128], rhs=Wo[:, cc, half * 512:(half + 1) * 512], start=(cc == 0), stop=(cc == 7)), reads=[mT, Wo], writes=[px])
                    A("dve", lambda e: e.tensor_tensor(out=acc[lt][:, half * 512:(half + 1) * 512], in0=px[:], in1=X[s][:, half * 512:(half + 1) * 512], op=ALU.add), reads=[px, X[s]], writes=[acc[lt]])
                rmsnorm_stats(acc[lt][:], acc[lt])
                A("dve", lambda e: e.tensor_scalar(out=hnf[:], in0=acc[lt][:], scalar1=st1[:, 2:3], scalar2=None, op0=ALU.mult), reads=[acc[lt], st1], writes=[hnf])
                for g2 in range(2):
                    pt = PB[4 + g2]
                    for c4 in range(4):
                        c = g2 * 4 + c4
                        A("pe", lambda e: e.transpose(out=pt[:, c4 * 128:(c4 + 1) * 128], in_=hnf[:, c * 128:(c + 1) * 128], identity=idf[:]), reads=[hnf, idf], writes=[pt])
                    A("dve", lambda e: e.tensor_tensor(out=hTf[:, g2 * 4:(g2 + 1) * 4, :], in0=pt[:].rearrange("p (a b) -> p a b", a=4),
                                                      in1=bc(wn_ffn[:, g2 * 4:(g2 + 1) * 4].unsqueeze(2), [128, 4, 128]), op=ALU.mult), reads=[pt, prm], writes=[hTf])
                A("pool", lambda e: e.tensor_copy(out=hT[:, :, lt * 128:(lt + 1) * 128], in_=hTf[:]), reads=[hTf], writes=[hT])
                if moe:
                    pr = PB[6]
                    for c in range(8):
                        A("pe", lambda e: e.matmul(pr[:, 0:8], lhsT=hTf[:, c, :], rhs=Rw[:, c, :], start=(c == 0), stop=(c == 7)), reads=[hTf, Rw], writes=[pr])
                    lg, eq1, l2, eq2 = rt_[:, 0:8], rt_[:, 8:16], rt_[:, 16:24], rt_[:, 24:32]
                    m1, m2, dm, ex, g1, g2_ = rt_[:, 32:33], rt_[:, 33:34], rt_[:, 34:35], rt_[:, 35:36], rt_[:, 36:37], rt_[:, 37:38]
                    R_ = dict(reads=[rt_], writes=[rt_])
                    A("dve", lambda e: e.tensor_tensor(out=lg, in0=pr[:, 0:8], in1=rb, op=ALU.add), reads=[pr, prm], writes=[rt_])
                    A("dve", lambda e: e.tensor_reduce(out=m1, in_=lg, axis=AX.X, op=ALU.max), **R_)
                    A("dve", lambda e: e.tensor_scalar(out=eq1, in0=lg, scalar1=m1, scalar2=None, op0=ALU.is_equal), **R_)
                    A("dve", lambda e: e.scalar_tensor_tensor(out=l2, in0=eq1, scalar=-1e30, in1=lg, op0=ALU.mult, op1=ALU.add), **R_)
                    A("dve", lambda e: e.tensor_reduce(out=m2, in_=l2, axis=AX.X, op=ALU.max), **R_)
                    A("dve", lambda e: e.tensor_scalar(out=eq2, in0=l2, scalar1=m2, scalar2=None, op0=ALU.is_equal), **R_)
                    A("dve", lambda e: e.tensor_tensor(out=dm, in0=m2, in1=m1, op=ALU.subtract), **R_)
                    A("act", lambda e: e.activation(out=ex, in_=dm, func=AF.Exp), **R_)
                    A("dve", lambda e: e.tensor_scalar(out=g1, in0=ex, scalar1=1.0, scalar2=None, op0=ALU.add), **R_)
                    A("dve", lambda e: e.reciprocal(out=g1, in_=g1), **R_)
                    A("dve", lambda e: e.tensor_tensor(out=g2_, in0=ex, in1=g1, op=ALU.mult), **R_)
                    A("dve", lambda e: e.tensor_scalar(out=eq1, in0=eq1, scalar1=g1, scalar2=None, op0=ALU.mult), **R_)
                    A("dve", lambda e: e.scalar_tensor_tensor(out=gates[:, lt, :], in0=eq2, scalar=g2_, in1=eq1, op0=ALU.mult, op1=ALU.add), reads=[rt_], writes=[gates])

        alias_barrier(ffv, b1v)
        ki = 0
        for ex_ in range(E):
            for k in range(nchunk):
                f0 = k * 512
                fw = min(512, F - f0)
                nsub = fw // 128
                Wgs, Wds = Wgu[ki % 2], Wd[ki % 2]
                ki += 1
                A("pool", lambda e: e.dma_start(out=Wgs[:, :, 0:fw], in_=wgu_d[ex_, :, f0:f0 + fw].rearrange("(c p) n -> p c n", p=128)), writes=[Wgs], dma=Wgs)
                A("pool", lambda e: e.dma_start(out=Wgs[:, :, 512:512 + fw], in_=wgu_d[ex_, :, F + f0:F + f0 + fw].rearrange("(c p) n -> p c n", p=128)), writes=[Wgs], dma=Wgs)
                A("pool", lambda e: e.dma_start(out=Wds[:, 0:nsub, :], in_=wd_d[ex_, f0:f0 + fw, :].rearrange("(s p) n -> p s n", p=128)), writes=[Wds], dma=Wds)
                for tt in range(2):
                    aT = actT[tt % 2]
                    for sub in range(nsub):
                        pg, pu = PB[(sub % 2) * 2], PB[(sub % 2) * 2 + 1]
                        for c in range(8):
                            A("pe", lambda e: e.matmul(pg[:], lhsT=Wgs[:, c, sub * 128:(sub + 1) * 128], rhs=hT[:, c, tt * 512:(tt + 1) * 512], start=(c == 0), stop=(c == 7)), reads=[Wgs, hT], writes=[pg])
                        for c in range(8):
                            A("pe", lambda e: e.matmul(pu[:], lhsT=Wgs[:, c, 512 + sub * 128:512 + (sub + 1) * 128], rhs=hT[:, c, tt * 512:(tt + 1) * 512], start=(c == 0), stop=(c == 7)), reads=[Wgs, hT], writes=[pu])
                        sg = tf[sub % 2]
                        tg = tf[2 + sub % 2]
                        sigmoid_from_psum(pg, sg)
                        A("dve", lambda e: e.tensor_tensor(out=tg[:], in0=pg[:], in1=sg[:], op=ALU.mult), reads=[pg, sg], writes=[tg])
                        A("dve", lambda e: e.tensor_tensor(out=aT[:, sub, :], in0=tg[:], in1=pu[:], op=ALU.mult), reads=[tg, pu], writes=[aT])
                    for s in range(4):
                        lt = tt * 4 + s
                        for half in range(2):
                            pd = PB[4 + ((s * 2 + half) % 4)]
                            for sub in range(nsub):
                                A("pe", lambda e: e.matmul(pd[:], lhsT=aT[:, sub, s * 128:(s + 1) * 128], rhs=Wds[:, sub, half * 512:(half + 1) * 512], start=(sub == 0), stop=(sub == nsub - 1)), reads=[aT, Wds], writes=[pd])
                            dst = acc[lt][:, half * 512:(half + 1) * 512]
                            if moe:
                                A("dve", lambda e: e.scalar_tensor_tensor(out=dst, in0=pd[:], scalar=gates[:, lt, ex_:ex_ + 1], in1=dst, op0=ALU.mult, op1=ALU.add), reads=[pd, gates, acc[lt]], writes=[acc[lt]])
                            else:
                                A("dve", lambda e: e.tensor_tensor(out=dst, in0=pd[:], in1=dst, op=ALU.add), reads=[pd, acc[lt]], writes=[acc[lt]])
        for lt in range(8):
            r0 = hp * PASS + lt * 128
            A("sp", lambda e: e.dma_start(out=out_d[r0:r0 + 128, :], in_=acc[lt][:]), reads=[acc[lt]], dma=acc[lt])

    P.emit(final_bufs=[a.b for a in acc])
    return P


def host_inputs_B(inp, layer, moe):
    w_in = inp["w_in"][layer]
    d = {
        "wg": np.ascontiguousarray(w_in[:, 3592:5640]),
        "wa": np.ascontiguousarray(inp["w_branch_a"][layer]),
        "wb": np.ascontiguousarray(inp["w_branch_b"][layer]),
        "wo": np.ascontiguousarray(inp["w_out"][layer]),
        "idn": np.eye(128, dtype=np.float32),
    }
    prm = np.zeros((128, NPRMB), np.float32)
    prm[:, 0:8] = inp["norm_mix_w"][layer].reshape(8, 128).T
    prm[:, 8:16] = inp["norm_ffn_w"][layer].reshape(8, 128).T
    j = layer // 2
    if moe:
        prm[:, 16:24] = inp["router_b"][j][None, :]
        d["rw"] = np.ascontiguousarray(inp["router_w"][j])
        d["wgu"] = inp["moe_w_gate_up"][j]
        d["wd"] = inp["moe_w_down"][j]
    else:
        d["wgu"] = inp["ffn_w_gate_up"][j][None]
        d["wd"] = inp["ffn_w_down"][j][None]
    d["prm"] = prm
    return d


from concourse.bass_utils import run_bass_kernel_spmd

_S = 8192
_NTOK = 4096


def _run(nc, maps):
    return run_bass_kernel_spmd(nc, maps, core_ids=list(range(8))).results


def kernel(**inp):
    inp = {k: np.asarray(v) for k, v in inp.items()}
    cur = np.ascontiguousarray(inp["x"], dtype=np.float32)
    for layer in range(2):
        lam_init = 0.8 - 0.6 * math.exp(-0.3 * layer)
        moe = (layer % 2 == 1)
        nc = bass.Bass("TRN2", target_bir_lowering=False)
        build_A(nc, _S, lam_init)
        maps = []
        for core in range(8):
            b, hh = core // 2, core % 2
            d = host_inputs_A(inp, layer, b, hh, _S)
            d["x"] = cur[b]
            maps.append(d)
        resA = _run(nc, maps)
        del nc
        nc = bass.Bass("TRN2", target_bir_lowering=False)
        build_B(nc, _NTOK, moe, 3584 if moe else 2816, 8 if moe else 1)
        wts = host_inputs_B(inp, layer, moe)
        maps = []
        for core in range(8):
            b, half = core // 2, core % 2
            sl = slice(half * _NTOK, (half + 1) * _NTOK)
            d = dict(wts)
            d["x"] = np.ascontiguousarray(cur[b, sl])
            d["oaT"] = np.ascontiguousarray(np.concatenate([resA[2 * b]["oaT"][:, :, sl], resA[2 * b + 1]["oaT"][:, :, sl]], axis=0))
            d["obT"] = np.ascontiguousarray(np.concatenate([resA[2 * b]["obT"][:, :, sl], resA[2 * b + 1]["obT"][:, :, sl]], axis=0))
            maps.append(d)
        resB = _run(nc, maps)
        del nc
        nxt = np.empty_like(cur)
        for core in range(8):
            b, half = core // 2, core % 2
            nxt[b, half * _NTOK:(half + 1) * _NTOK] = resB[core]["xo"]
        cur = nxt
    return cur
```
